# Optimizing a Trainium2 kernel written in Bass

```python
import jax, jax.numpy as jnp
from jax import lax
import numpy as np

D_MODEL = 1024
BATCH = 4
SEQ = 4096
DEPTH = 2

D_POOL = D_MODEL // 2
POOL_WINDOWS = (2, 4, 8, 16)
N_POOL_GROUPS = len(POOL_WINDOWS)
POOL_GROUP = D_POOL // N_POOL_GROUPS
D_ATTN = D_MODEL // 2
HEAD_DIM = 64
N_HEADS = D_ATTN // HEAD_DIM
DILATED_PATTERNS = ((128, 1), (512, 4), (2048, 16))
Q_BLOCK = 128
D_IN = D_POOL + 3 * D_ATTN
D_FF = 2816
N_EXPERTS = 8
TOP_K = 2
D_FF_EXPERT = 3584
EPS = 1e-6
N_DENSE = (DEPTH + 1) // 2
N_MOE = DEPTH // 2

kernel_name = "hybrid_pool_dilated_attn_moe_block"


def rmsnorm(x, g):
    xf = x.astype(jnp.float32)
    y = xf * lax.rsqrt(jnp.mean(xf * xf, axis=-1, keepdims=True) + EPS)
    return (y * g.astype(jnp.float32)).astype(x.dtype)


def pool_mixer(u, w_pool, pool_scale):
    B, S, _ = u.shape
    uf = u.astype(jnp.float32)
    c = jnp.concatenate([jnp.zeros((B, 1, D_POOL), jnp.float32), jnp.cumsum(uf, axis=1)], axis=1)
    t = jnp.arange(S)
    diffs = []
    for gi, w in enumerate(POOL_WINDOWS):
        sl = slice(gi * POOL_GROUP, (gi + 1) * POOL_GROUP)
        cg = c[..., sl]
        lo = jnp.maximum(t + 1 - w, 0)
        win_sum = cg[:, 1:] - cg[:, lo]
        cnt = (t + 1 - lo).astype(jnp.float32)[None, :, None]
        diffs.append(win_sum / cnt - uf[..., sl])
    d = jnp.stack(diffs, axis=2).astype(u.dtype)
    z = jnp.einsum('bsgc,gce->bsge', d, w_pool).reshape(B, S, D_POOL)
    return rmsnorm(z, pool_scale)


def dilated_attention(q, k, v):
    B, H, S, Dh = q.shape
    nb = S // Q_BLOCK
    scale = HEAD_DIM ** -0.5
    qb = q.reshape(B, H, nb, Q_BLOCK, Dh).transpose(2, 0, 1, 3, 4)

    def block(args):
        bi, qblk = args
        pos = bi * Q_BLOCK + jnp.arange(Q_BLOCK)
        outs, lses = [], []
        for window, dil in DILATED_PATTERNS:
            n_keys = window // dil + 1
            kidx = pos[:, None] - dil * jnp.arange(n_keys)[None, :]
            valid = kidx >= 0
            kidx = jnp.maximum(kidx, 0)
            kg = k[:, :, kidx]
            vg = v[:, :, kidx]
            s = jnp.einsum('bhqd,bhqjd->bhqj', qblk, kg).astype(jnp.float32) * scale
            s = jnp.where(valid[None, None], s, -jnp.inf)
            m = jnp.max(s, axis=-1, keepdims=True)
            p = jnp.exp(s - m)
            den = jnp.sum(p, axis=-1, keepdims=True)
            o = jnp.einsum('bhqj,bhqjd->bhqd', p, vg.astype(jnp.float32)) / den
            outs.append(o)
            lses.append(m + jnp.log(den))
        wts = jax.nn.softmax(jnp.concatenate(lses, axis=-1), axis=-1)
        o = jnp.einsum('bhqp,pbhqd->bhqd', wts, jnp.stack(outs, axis=0))
        return o.astype(q.dtype)

    ob = lax.map(block, (jnp.arange(nb), qb))
    return ob.transpose(1, 2, 0, 3, 4).reshape(B, H, S, Dh)


def hybrid_mixer(h, w_in, w_pool, pool_scale, attn_gain, w_out):
    B, S, _ = h.shape
    proj = h @ w_in
    u = proj[..., :D_POOL]
    q, k, v = jnp.split(proj[..., D_POOL:], 3, axis=-1)
    to_heads = lambda t: t.reshape(B, S, N_HEADS, HEAD_DIM).transpose(0, 2, 1, 3)
    y_a = pool_mixer(u, w_pool, pool_scale)
    o = dilated_attention(to_heads(q), to_heads(k), to_heads(v))
    y_b = rmsnorm(o.transpose(0, 2, 1, 3).reshape(B, S, D_ATTN), attn_gain)
    return jnp.concatenate([y_a, y_b], axis=-1) @ w_out


def swiglu(h, wg, wu, wd):
    return (jax.nn.silu(h @ wg) * (h @ wu)) @ wd


def moe_swiglu(h, w_router, wg, wu, wd):
    B, S, D = h.shape
    xt = h.reshape(-1, D)
    logits = (xt @ w_router).astype(jnp.float32)
    top_v, top_i = lax.top_k(logits, TOP_K)
    gates = jax.nn.softmax(top_v, axis=-1)
    gate = jnp.sum(jax.nn.one_hot(top_i, N_EXPERTS, dtype=jnp.float32) * gates[..., None], axis=1)
    y = jnp.zeros_like(xt)
    for e in range(N_EXPERTS):
        y = y + gate[:, e:e + 1].astype(xt.dtype) * swiglu(xt, wg[e], wu[e], wd[e])
    return y.reshape(B, S, D)


def setup_inputs(seed: int = 0) -> dict:
    key = jax.random.key(seed)
    ks = jax.random.split(key, 20)
    f32 = jnp.float32
    nrm = lambda k, shape, fan_in: jax.random.normal(k, shape, f32) * (fan_in ** -0.5)
    gain = lambda k, shape: 1.0 + 0.05 * jax.random.normal(k, shape, f32)
    return {
        "x": jax.random.normal(ks[0], (BATCH, SEQ, D_MODEL), f32),
        "norm_mix": gain(ks[1], (DEPTH, D_MODEL)),
        "w_in": nrm(ks[2], (DEPTH, D_MODEL, D_IN), D_MODEL),
        "w_pool": nrm(ks[3], (DEPTH, N_POOL_GROUPS, POOL_GROUP, POOL_GROUP), POOL_GROUP),
        "pool_scale": gain(ks[4], (DEPTH, D_POOL)),
        "attn_gain": gain(ks[5], (DEPTH, D_ATTN)),
        "w_out": nrm(ks[6], (DEPTH, D_MODEL, D_MODEL), D_MODEL),
        "norm_ffn": gain(ks[7], (DEPTH, D_MODEL)),
        "ffn_wg": nrm(ks[8], (N_DENSE, D_MODEL, D_FF), D_MODEL),
        "ffn_wu": nrm(ks[9], (N_DENSE, D_MODEL, D_FF), D_MODEL),
        "ffn_wd": nrm(ks[10], (N_DENSE, D_FF, D_MODEL), D_FF),
        "w_router": nrm(ks[11], (N_MOE, D_MODEL, N_EXPERTS), D_MODEL),
        "moe_wg": nrm(ks[12], (N_MOE, N_EXPERTS, D_MODEL, D_FF_EXPERT), D_MODEL),
        "moe_wu": nrm(ks[13], (N_MOE, N_EXPERTS, D_MODEL, D_FF_EXPERT), D_MODEL),
        "moe_wd": nrm(ks[14], (N_MOE, N_EXPERTS, D_FF_EXPERT, D_MODEL), D_FF_EXPERT),
        "final_norm": gain(ks[15], (D_MODEL,)),
    }


def reference(x, norm_mix, w_in, w_pool, pool_scale, attn_gain, w_out, norm_ffn,
              ffn_wg, ffn_wu, ffn_wd, w_router, moe_wg, moe_wu, moe_wd, final_norm):
    for l in range(DEPTH):
        h = rmsnorm(x, norm_mix[l])
        x = x + hybrid_mixer(h, w_in[l], w_pool[l], pool_scale[l], attn_gain[l], w_out[l])
        h = rmsnorm(x, norm_ffn[l])
        if l % 2 == 0:
            i = l // 2
            x = x + swiglu(h, ffn_wg[i], ffn_wu[i], ffn_wd[i])
        else:
            i = l // 2
            x = x + moe_swiglu(h, w_router[i], moe_wg[i], moe_wu[i], moe_wd[i])
    return rmsnorm(x, final_norm)
```

```python
import numpy as np
import ml_dtypes
import concourse.bass as bass
import concourse.mybir as mybir
from concourse.bass_utils import run_bass_kernel_spmd

F32 = mybir.dt.float32
BF16 = mybir.dt.bfloat16
ALU = mybir.AluOpType
AF = mybir.ActivationFunctionType
AX = mybir.AxisListType

D = 1024
SEQ = 4096
NT = 4096
OWN0 = 2048
G = 512
NG = NT // G
D_IN = 2048
D_FF = 2816
NE = 8
D_FFE = 3584
EPS = 1e-6
MASK_LO = 3
MASK_N = 23
NWARM0 = 8
NWARM1 = 1
NB2 = 3


class Tr:
    EPOCH = 12000

    def __init__(self, nc):
        self.nc = nc
        self.eng = {"pe": nc.tensor, "act": nc.scalar, "dve": nc.vector, "pool": nc.gpsimd, "sp": nc.sync}
        self.cnt = {e: 0 for e in self.eng}
        self.nsem = 0
        self.sem = {e: self.new_sem(e) for e in self.eng}
        self.own = {e: {id(self.sem[e])} for e in self.eng}
        self.semobj = {}
        self.lw = {}
        self.rd = {}
        self.waited = {e: {} for e in self.eng}
        self.dsem = {}
        self.dcnt = {}

    def new_sem(self, name):
        self.nsem += 1
        return self.nc.alloc_semaphore(f"s_{name}_{self.nsem}")

    def _deps(self, e, reads, writes):
        deps = []
        for k in list(reads) + list(writes):
            ev = self.lw.get(k)
            if ev is not None:
                deps.append((ev, True))
        for k in writes:
            for ev in self.rd.get(k, {}).values():
                deps.append((ev, False))
        out = {}
        for (ev, is_w) in deps:
            sem, val, src = ev
            if src == e:
                if e in ("pe", "sp"):
                    continue
                if not is_w:
                    continue
            key = id(sem)
            if self.waited[e].get(key, 0) >= val:
                continue
            if key not in out or out[key][1] < val:
                out[key] = (sem, val)
        for key, (sem, val) in out.items():
            self.eng[e].wait_ge(sem, val)
            self.waited[e][key] = val

    def _record(self, ev, reads, writes):
        for k in writes:
            self.lw[k] = ev
            self.rd[k] = {}
        for k in reads:
            self.rd.setdefault(k, {})[id(ev[0])] = ev

    def op(self, e, fn, reads=(), writes=()):
        self._deps(e, reads, writes)
        ins = fn(self.eng[e])
        self.cnt[e] += 1
        ins.then_inc(self.sem[e], 1)
        ev = (self.sem[e], self.cnt[e], e)
        self._record(ev, reads, writes)
        if self.cnt[e] >= self.EPOCH:
            self.sem[e] = self.new_sem(e)
            self.cnt[e] = 0
        return ev

    def dma(self, q, out, in_, reads=(), writes=(), semkey=None):
        self._deps(q, reads, writes)
        if semkey not in self.dsem:
            self.dsem[semkey] = self.new_sem("d")
            self.dcnt[semkey] = 0
        sem = self.dsem[semkey]
        self.eng[q].dma_start(out=out, in_=in_).then_inc(sem, 16)
        self.dcnt[semkey] += 16
        ev = (sem, self.dcnt[semkey], "dma")
        self._record(ev, reads, writes)
        return ev

    def barrier(self):
        evs = [(self.sem[e], self.cnt[e]) for e in self.eng if self.cnt[e] > 0]
        evs += [(self.dsem[k], self.dcnt[k]) for k in self.dsem if self.dcnt[k] > 0]
        for e in self.eng:
            for sem, val in evs:
                if sem is self.sem[e]:
                    continue
                if self.waited[e].get(id(sem), 0) >= val:
                    continue
                self.eng[e].wait_ge(sem, val)
                self.waited[e][id(sem)] = val

    def wait_all_dma(self, q, keys):
        for k in keys:
            if k in self.dsem and self.dcnt[k] > 0:
                self.eng[q].wait_ge(self.dsem[k], self.dcnt[k])


def mult_mask_table():
    kk = np.arange(128)[:, None]
    qq = np.arange(128)[None, :]
    tab = np.zeros((128, MASK_N * 128), np.float32)
    for di in range(MASK_N):
        delta = di - MASK_LO
        off = delta * 128 + qq - kk
        m = np.zeros_like(off, dtype=np.float32)
        m += ((off >= 0) & (off <= 128)).astype(np.float32)
        m += ((off >= 0) & (off <= 512) & (off % 4 == 0)).astype(np.float32)
        m += ((off >= 0) & (off <= 2048) & (off % 16 == 0)).astype(np.float32)
        tab[:, di * 128:(di + 1) * 128] = m
    return tab.astype(ml_dtypes.bfloat16)


class K:
    pass


def build(phases=("m0", "f0", "m1", "f1"), debug=False):
    nc = bass.Bass("TRN2", target_bir_lowering=False)
    k = K()
    k.nc = nc
    k.tr = Tr(nc)
    tr = k.tr
    dt_in = lambda name, shape, dt=F32: nc.dram_tensor(name, list(shape), dt, kind="ExternalInput").ap()
    k.x_loc = dt_in("x_loc", [NT, D])
    k.norm_mix = dt_in("norm_mix", [2, D])
    k.w_in = dt_in("w_in", [2, D, D_IN])
    k.w_pool = dt_in("w_pool", [2, 4, 128, 128])
    k.pool_scale = dt_in("pool_scale", [2, 512])
    k.attn_gain = dt_in("attn_gain", [2, 512])
    k.w_out = dt_in("w_out", [2, D, D])
    k.norm_ffn = dt_in("norm_ffn", [2, D])
    k.ffn_wg = dt_in("ffn_wg", [1, D, D_FF])
    k.ffn_wu = dt_in("ffn_wu", [1, D, D_FF])
    k.ffn_wd = dt_in("ffn_wd", [1, D_FF, D])
    k.w_router = dt_in("w_router", [1, D, NE])
    k.moe_wg = dt_in("moe_wg", [1, NE, D, D_FFE])
    k.moe_wu = dt_in("moe_wu", [1, NE, D, D_FFE])
    k.moe_wd = dt_in("moe_wd", [1, NE, D_FFE, D])
    k.final_norm = dt_in("final_norm", [1, D])
    k.maskT = dt_in("maskT", [128, MASK_N * 128], BF16)
    k.ident = dt_in("ident", [128, 128], BF16)
    k.identf = dt_in("identf", [128, 128], F32)
    k.rc16 = dt_in("rc16", [2, 64])
    k.cflag = dt_in("cflag", [128, 1])
    k.out = nc.dram_tensor("out", [NT - OWN0, D], F32, kind="ExternalOutput").ap()
    skind = "ExternalOutput" if debug else "Internal"
    k.xa = nc.dram_tensor("xa", [NT, D], F32, kind=skind).ap()
    k.xb = nc.dram_tensor("xb", [NT, D], F32, kind=skind).ap()
    k.xc = nc.dram_tensor("xc", [NT, D], F32, kind=skind).ap()
    bufs = {"x_loc": k.x_loc, "xa": k.xa, "xb": k.xb, "xc": k.xc, "out": k.out}
    sb = lambda name, shape, dt: nc.alloc_sbuf_tensor(name, list(shape), dt)
    ps = lambda name, shape, dt: nc.alloc_psum_tensor(name, list(shape), dt)
    k.psall = ps("psall", [128, 7, 512], F32)
    k.ps = [k.psall[:, i, :] for i in range(7)]
    k.psT = ps("psT", [128, 8, 128], BF16)
    k.mt = sb("mt", [128, MASK_N * 128], BF16)
    k.idb = sb("idb", [128, 128], BF16)
    k.idf = sb("idf", [128, 128], F32)
    k.ones = sb("ones", [128, 128], BF16)
    k.cfl = sb("cfl", [128, 1], F32)
    k.rc = sb("rc", [128, 2, 64], F32)
    tr.dma("sp", k.mt[:], k.maskT, writes=["mt"], semkey="const")
    tr.dma("sp", k.idb[:], k.ident, writes=["idb"], semkey="const")
    tr.dma("sp", k.idf[:], k.identf, writes=["idf"], semkey="const")
    tr.dma("sp", k.cfl[:], k.cflag, writes=["cfl"], semkey="const")
    tr.dma("sp", k.rc[:].rearrange("p a b -> p (a b)"),
           k.rc16.rearrange("a b -> (a b)").partition_broadcast(128), writes=["rc"], semkey="const")
    tr.op("dve", lambda e: e.memset(k.ones[:], 1.0), writes=["ones"])
    for (kind, l, src, dst, lo, hi) in phases:
        if kind == "m":
            mixer_phase(k, l, bufs[src], bufs[dst], lo, hi)
        else:
            ffn_phase(k, l, bufs[src], bufs[dst], lo, hi)
        tr.barrier()
    tr.wait_all_dma("sp", list(tr.dsem.keys()))
    return nc


FULL_PHASES = (("m", 0, "x_loc", "xa", 0, NG), ("f", 0, "xa", "xb", 0, NT),
               ("m", 1, "xb", "xc", NG // 2, NG), ("f", 1, "xc", "out", OWN0, NT))


def _ap2(base_ap, stride):
    return bass.AP(base_ap.tensor, base_ap.offset, [list(base_ap.ap[0]), [stride, 2], [1, 64]])


def rms_rstd(tr, st, n, kp=""):
    tr.op("act", lambda e: e.activation(out=st[:, 1:2], in_=st[:, 0:1], func=AF.Ln, scale=1.0 / n, bias=EPS),
          reads=[kp + "st0"], writes=[kp + "st1"])
    tr.op("act", lambda e: e.activation(out=st[:, 2:3], in_=st[:, 1:2], func=AF.Exp, scale=-0.5),
          reads=[kp + "st1"], writes=[kp + "st2"])


def rstd_tile(tr, rstd, psbank, pskey, n):
    tr.op("act", lambda e: e.activation(out=rstd[:, :], in_=psbank[:, :], func=AF.Ln, scale=1.0 / n, bias=EPS),
          reads=[pskey], writes=["rstd"])
    tr.op("act", lambda e: e.activation(out=rstd[:, :], in_=rstd[:, :], func=AF.Exp, scale=-0.5),
          reads=["rstd"], writes=["rstd"])


def mixer_phase(k, l, X_in, X_out, g_lo, g_hi):
    from contextlib import ExitStack
    nc, tr = k.nc, k.tr
    pf = f"m{l}_"
    with ExitStack() as es:
        sbt = lambda name, shape, dt: es.enter_context(nc.sbuf_tensor(pf + name, list(shape), dt))
        w_in = sbt("w_in", [128, 8, D_IN], BF16)
        w_out = sbt("w_out", [128, 8, D], BF16)
        w_pool = sbt("w_pool", [128, 4, 128], BF16)
        gmix = sbt("gmix", [128, D], F32)
        pscale = sbt("pscale", [128, 4], F32)
        again = sbt("again", [128, 4], F32)
        kT = sbt("kT", [128, 4, 5 * G], BF16)
        vr = sbt("vr", [128, 20, 8, 128], BF16)
        xt = [sbt(f"xt{i}", [128, D], F32) for i in range(2)]
        hb = [sbt(f"hb{i}", [128, D], BF16) for i in range(2)]
        st2 = sbt("st2", [128, 2, 8], F32)
        hT = sbt("hT", [128, 8, G], BF16)
        uT = sbt("uT", [128, 4, 16 + G], F32)
        sA = sbt("sA", [128, 16 + G], F32)
        sB = sbt("sB", [128, 16 + G], F32)
        tmp16 = sbt("tmp16", [128, 16], F32)
        dT = sbt("dT", [128, 4, G], BF16)
        qT = sbt("qT", [128, 4, G], BF16)
        zo = sbt("zo", [128, 4, G], F32)
        sq = sbt("sq", [128, 4, G], BF16)
        rstd = sbt("rstd", [128, G], F32)
        yT = sbt("yT", [128, 8, G], BF16)
        NPB = 4
        pT = [sbt(f"pT{i}", [128, 2, G], BF16) for i in range(NPB)]
        S2 = [k.psall[:, 2:4, :], k.psall[:, 0:2, :]]
        S2K = [["ps2", "ps3"], ["ps0", "ps1"]]
        SKEW = 2
        rz = sbt("rz", [128, G], F32)
        xr = [sbt(f"xr{i}", [128, D], F32) for i in range(2)]
        PS = k.ps
        psT = k.psT

        for c in range(8):
            tr.dma("pool", w_in[:, c, :], k.w_in[l, c * 128:(c + 1) * 128, :], writes=["w_in"], semkey="mw")
        for c in range(8):
            tr.dma("pool", w_out[:, c, :], k.w_out[l, c * 128:(c + 1) * 128, :], writes=["w_out"], semkey="mw")
        tr.dma("pool", w_pool[:], k.w_pool[l], writes=["w_pool"], semkey="mw")
        tr.dma("sp", gmix[:], k.norm_mix[l:l + 1, :].partition_broadcast(128), writes=["gmix"], semkey="mc")
        tr.dma("sp", pscale[:], k.pool_scale[l], writes=["pscale"], semkey="mc")
        tr.dma("sp", again[:], k.attn_gain[l], writes=["again"], semkey="mc")
        vkeys = [f"vr{i}" for i in range(20)]
        tr.op("pool", lambda e: e.memset(vr[:, :, :, :], 1.0), writes=vkeys)
        tr.op("dve", lambda e: e.memset(uT[:, :, 0:16], 0.0), writes=[f"uT{j}" for j in range(4)])

        for g in range(NG):
            active = g_lo <= g < g_hi
            def stage1(t):
                tt = 4 * g + t
                xs, hs = xt[tt % 2], hb[tt % 2]
                xk, hk = f"xt{tt % 2}", f"hb{tt % 2}"
                kp = f"p{tt % 2}"
                stp = st2[:, tt % 2, :]
                tr.dma("sp", xs[:], X_in[tt * 128:(tt + 1) * 128, :], reads=[f"{X_in.name}.{tt}"], writes=[xk],
                       semkey=pf + xk)
                tr.op("dve", lambda e: e.memset(stp[:, 0:1], 0.0), writes=[kp + "st0"])
                tr.op("act", lambda e: e.activation(out=pT[NPB - 1][:].rearrange("p a b -> p (a b)"), in_=xs[:],
                                                    func=AF.Square, accum_out=stp[:, 0:1]),
                      reads=[xk], writes=[f"pT{NPB - 1}", kp + "st0"])
                rms_rstd(tr, stp, D, kp)
                tr.op("dve", lambda e: e.scalar_tensor_tensor(
                    out=hs[:], in0=xs[:], scalar=stp[:, 2:3], in1=gmix[:], op0=ALU.mult, op1=ALU.mult),
                    reads=[xk, kp + "st2", "gmix"], writes=[hk])

            def stage2(t):
                tt = 4 * g + t
                hs, hk = hb[tt % 2], f"hb{tt % 2}"
                for c in range(8):
                    tr.op("pe", lambda e, c=c: e.transpose(out=psT[:, c, :], in_=hs[:, c * 128:(c + 1) * 128],
                                                           identity=k.idb[:]),
                          reads=[hk, "idb"], writes=["psT"])
                tr.op("act", lambda e: e.copy(out=hT[:, :, t * 128:(t + 1) * 128], in_=psT[:, :, :]),
                      reads=["psT"], writes=[f"hT{t}"])

            for t in range(4):
                stage1(t)
                if t >= 1:
                    stage2(t - 1)
            stage2(3)
            hkeys = [f"hT{t}" for t in range(4)]
            def pool_windows():
                for gi in range(4):
                    w = 2 << gi
                    a = uT[:, gi, :]
                    chain = [(sA, "sA", 1, 1)]
                    if gi >= 1:
                        chain.append((sB, "sB", 3, 2))
                    if gi >= 2:
                        chain.append((sA, "sA", 7, 4))
                    if gi >= 3:
                        chain.append((sB, "sB", 15, 8))
                    src, srck = a, f"uT{gi}"
                    for (dst, dk, i0, sh) in chain:
                        tr.op("dve", lambda e, dst=dst, src=src, i0=i0, sh=sh: e.tensor_tensor(
                            out=dst[:, i0:16 + G], in0=src[:, i0:16 + G], in1=src[:, i0 - sh:16 + G - sh], op=ALU.add),
                            reads=[srck], writes=[dk])
                        src, srck = dst, dk
                    tr.op("dve", lambda e, src=src, a=a, gi=gi, w=w: e.scalar_tensor_tensor(
                        out=dT[:, gi, :], in0=src[:, 16:16 + G], scalar=1.0 / w, in1=a[:, 16:16 + G],
                        op0=ALU.mult, op1=ALU.subtract), reads=[srck, f"uT{gi}"], writes=[f"dT{gi}"])
                    if g == 0 or g == NG // 2:
                        which = 0 if g == 0 else 1
                        tr.op("dve", lambda e, src=src, gi=gi, which=which: e.tensor_tensor(
                            out=tmp16[:, :], in0=src[:, 16:32], in1=k.rc[:, which, gi * 16:(gi + 1) * 16], op=ALU.mult),
                            reads=[srck, "rc"], writes=["tmp16"])
                        tr.op("dve", lambda e, a=a, gi=gi: e.tensor_tensor(
                            out=dT[:, gi, 0:16], in0=tmp16[:, :], in1=a[:, 16:32], op=ALU.subtract),
                            reads=["tmp16", f"uT{gi}"], writes=[f"dT{gi}"])

            for j in range(12):
                if 4 <= j < 8 and not active:
                    continue
                bank, bk = PS[j % 2], f"ps{j % 2}"
                for c in range(8):
                    tr.op("pe", lambda e, c=c, j=j, bank=bank: e.matmul(
                        bank[:, :], lhsT=w_in[:, c, j * 128:(j + 1) * 128], rhs=hT[:, c, :],
                        start=(c == 0), stop=(c == 7)), reads=["w_in"] + hkeys, writes=[bk])
                if j < 4:
                    tr.op("act", lambda e, j=j, bank=bank: e.copy(out=uT[:, j, 16:16 + G], in_=bank[:, :]),
                          reads=[bk], writes=[f"uT{j}"])
                    if j == 3 and active:
                        pool_windows()
                elif j < 8:
                    tr.op("act", lambda e, j=j, bank=bank: e.activation(out=qT[:, j - 4, :], in_=bank[:, :],
                                                                       func=AF.Copy, scale=0.125),
                          reads=[bk], writes=[f"qT{j - 4}"])
                else:
                    s0 = (g % 5) * G
                    tr.op("dve", lambda e, j=j, bank=bank, s0=s0: e.tensor_copy(out=kT[:, j - 8, s0:s0 + G],
                                                                               in_=bank[:, :]),
                          reads=[bk], writes=[f"kT{j - 8}_{g % 5}"])
            for t in range(4):
                tt = 4 * g + t
                bank, bk = PS[t % 2], f"ps{t % 2}"
                for c in range(8):
                    tr.op("pe", lambda e, c=c, t=t, bank=bank: e.matmul(
                        bank[:, :], lhsT=hT[:, c, t * 128:(t + 1) * 128], rhs=w_in[:, c, 1536:2048],
                        start=(c == 0), stop=(c == 7)), reads=["w_in", f"hT{t}"], writes=[bk])
                bv = bank[:, :].rearrange("p (h d) -> p h d", d=64)
                tr.op("dve", lambda e, tt=tt, bv=bv: e.tensor_copy(out=vr[:, tt % 20, 0:8:2, 0:64], in_=bv[:, 0:8:2, :]),
                      reads=[bk], writes=[f"vr{tt % 20}"])
                tr.op("dve", lambda e, tt=tt, bv=bv: e.tensor_copy(out=vr[:, tt % 20, 1:8:2, 64:128], in_=bv[:, 1:8:2, :]),
                      reads=[bk], writes=[f"vr{tt % 20}"])
                if tt >= 20:
                    tr.op("dve", lambda e, tt=tt: e.memset(vr[:, tt % 20, 0:8:2, 64:128], 1.0), writes=[f"vr{tt % 20}"])
                    tr.op("dve", lambda e, tt=tt: e.memset(vr[:, tt % 20, 1:8:2, 0:64], 1.0), writes=[f"vr{tt % 20}"])
            if active:
                for gi in range(4):
                    bank, bk = PS[gi % 2], f"ps{gi % 2}"
                    tr.op("pe", lambda e, gi=gi, bank=bank: e.matmul(bank[:, :], lhsT=w_pool[:, gi, :], rhs=dT[:, gi, :],
                                                                     start=True, stop=True),
                          reads=["w_pool", f"dT{gi}"], writes=[bk])
                    tr.op("act", lambda e, gi=gi, bank=bank: e.copy(out=zo[:, gi, :], in_=bank[:, :]),
                          reads=[bk], writes=[f"zo{gi}"])
                    tr.op("act", lambda e, gi=gi, bank=bank: e.activation(out=sq[:, gi, :], in_=bank[:, :],
                                                                         func=AF.Square),
                          reads=[bk], writes=[f"sq{gi}"])

                def pool_norm():
                    for gi in range(4):
                        tr.op("pe", lambda e, gi=gi: e.matmul(PS[4][:, :], lhsT=k.ones[:, :], rhs=sq[:, gi, :],
                                                              start=(gi == 0), stop=(gi == 3)),
                              reads=["ones", f"sq{gi}"], writes=["ps4"])
                    rstd_tile(tr, rstd, PS[4], "ps4", 512)
                    for gi in range(4):
                        tr.op("dve", lambda e, gi=gi: e.scalar_tensor_tensor(
                            out=yT[:, gi, :], in0=zo[:, gi, :], scalar=pscale[:, gi:gi + 1], in1=rstd[:, :],
                            op0=ALU.mult, op1=ALU.mult), reads=[f"zo{gi}", "pscale", "rstd"], writes=[f"yT{gi}"])
            for j in range(4):
                tr.op("pool", lambda e, j=j: e.tensor_copy(out=uT[:, j, 0:16], in_=uT[:, j, G:G + 16]),
                      reads=[f"uT{j}"], writes=[f"uT{j}"])
            if not active:
                continue
            kt0 = max(0, 4 * g - 16)
            if g == NG // 2:
                for kt in range(0, OWN0 // 128):
                    tr.op("dve", lambda e, kt=kt: e.tensor_scalar(
                        out=vr[:, kt % 20, :, :], in0=vr[:, kt % 20, :, :], scalar1=k.cfl[:, 0:1], scalar2=None,
                        op0=ALU.mult), reads=[f"vr{kt % 20}", "cfl"], writes=[f"vr{kt % 20}"])
            for j in range(4):
                items = []
                for kt in range(kt0, 4 * g + 4, 2):
                    for hh in range(2):
                        items.append((kt + 1, kt, hh))
                XB = [PS[5], PS[6]]
                nit = len(items)
                first = [True, True]

                def qk(i):
                    kth, ktl, hh = items[i]
                    S, sks = S2[i % 2], S2K[i % 2]
                    p0 = 64 * hh
                    for half, kt in enumerate((kth, ktl)):
                        kg = kt // 4
                        ko = (kg % 5) * G + (kt % 4) * 128
                        tr.op("pe", lambda e, half=half, ko=ko: e.matmul(
                            S[:, half, :], lhsT=kT[p0:p0 + 64, j, ko:ko + 128], rhs=qT[p0:p0 + 64, j, :],
                            start=True, stop=True), reads=[f"kT{j}_{kg % 5}", f"qT{j}"], writes=[sks[half]])
                    pk = f"pT{i % NPB}"
                    pb = pT[i % NPB]
                    tr.op("act", lambda e: e.activation(out=pb[:, :, :], in_=S[:, :, :], func=AF.Exp),
                          reads=sks, writes=[pk])
                    di0 = (4 * g - kth) + MASK_LO
                    mb = k.mt[:, di0 * 128:di0 * 128 + G]
                    msk = bass.AP(mb.tensor, mb.offset, [list(mb.ap[0]), [128, 2], [1, G]])
                    tr.op("dve", lambda e: e.tensor_tensor(out=pb[:, :, :], in0=pb[:, :, :], in1=msk, op=ALU.mult),
                          reads=[pk, "mt"], writes=[pk])

                def pv(i):
                    kth, ktl, hh = items[i]
                    h = 2 * j + hh
                    X = XB[hh]
                    for half, kt in enumerate((kth, ktl)):
                        slot = kt % 20
                        st_ = first[hh]
                        first[hh] = False
                        tr.op("pe", lambda e, half=half, slot=slot, st_=st_, kt=kt: e.matmul(
                            X[:, :], lhsT=vr[:, slot, h, :], rhs=pT[i % NPB][:, half, :],
                            start=st_, stop=(kt == 4 * g + 3), skip_group_check=True),
                            reads=[f"vr{slot}", f"pT{i % NPB}"], writes=[f"ps{5 + hh}"])

                def warm(n):
                    for _ in range(n):
                        tr.op("pe", lambda e: e.matmul(PS[4][:, :], lhsT=k.ones[:, :], rhs=k.mt[:, 0:G],
                                                       start=True, stop=True), reads=["ones", "mt"], writes=["ps4"])
                if NWARM0:
                    warm(NWARM0)
                for i in range(nit + SKEW):
                    if i < nit:
                        qk(i)
                        if NWARM1:
                            warm(NWARM1)
                    if i >= SKEW:
                        pv(i - SKEW)
                if j == 0:
                    pool_norm()
                tr.op("act", lambda e: e.activation(out=rz[64:128, :], in_=PS[5][64:128, :], func=AF.Ln),
                      reads=["ps5"], writes=["rzA"])
                tr.op("act", lambda e: e.activation(out=rz[64:128, :], in_=rz[64:128, :], func=AF.Exp, scale=-1.0),
                      reads=["rzA"], writes=["rzA"])
                tr.op("dve", lambda e: e.tensor_tensor(out=zo[0:64, j, :], in0=PS[5][0:64, :], in1=rz[64:128, :],
                                                       op=ALU.mult), reads=["ps5", "rzA"], writes=[f"zo{j}"])
                tr.op("act", lambda e: e.activation(out=rz[0:64, :], in_=PS[6][0:64, :], func=AF.Ln),
                      reads=["ps6"], writes=["rzB"])
                tr.op("act", lambda e: e.activation(out=rz[0:64, :], in_=rz[0:64, :], func=AF.Exp, scale=-1.0),
                      reads=["rzB"], writes=["rzB"])
                tr.op("dve", lambda e: e.tensor_tensor(out=zo[64:128, j, :], in0=PS[6][64:128, :], in1=rz[0:64, :],
                                                       op=ALU.mult), reads=["ps6", "rzB"], writes=[f"zo{j}"])
                tr.op("act", lambda e: e.activation(out=sq[:, j, :], in_=zo[:, j, :], func=AF.Square),
                      reads=[f"zo{j}"], writes=[f"sq{j}"])
            for j in range(4):
                tr.op("pe", lambda e, j=j: e.matmul(PS[0][:, :], lhsT=k.ones[:, :], rhs=sq[:, j, :],
                                                    start=(j == 0), stop=(j == 3)),
                      reads=["ones", f"sq{j}"], writes=["ps0"])
            rstd_tile(tr, rstd, PS[0], "ps0", 512)
            for j in range(4):
                tr.op("dve", lambda e, j=j: e.scalar_tensor_tensor(
                    out=yT[:, 4 + j, :], in0=zo[:, j, :], scalar=again[:, j:j + 1], in1=rstd[:, :],
                    op0=ALU.mult, op1=ALU.mult), reads=[f"zo{j}", "again", "rstd"], writes=[f"yT{4 + j}"])
            ykeys = [f"yT{c}" for c in range(8)]
            for t in range(4):
                tt = 4 * g + t
                xs, xk = xr[tt % 2], f"xr{tt % 2}"
                tr.dma("sp", xs[:], X_in[tt * 128:(tt + 1) * 128, :], reads=[f"{X_in.name}.{tt}"], writes=[xk],
                       semkey=pf + xk)
                for half in range(2):
                    bank, bk = PS[half], f"ps{half}"
                    for c in range(8):
                        tr.op("pe", lambda e, c=c, t=t, half=half, bank=bank: e.matmul(
                            bank[:, :], lhsT=yT[:, c, t * 128:(t + 1) * 128], rhs=w_out[:, c, half * 512:(half + 1) * 512],
                            start=(c == 0), stop=(c == 7)), reads=["w_out"] + ykeys, writes=[bk])
                    tr.op("dve", lambda e, xs=xs, half=half, bank=bank: e.tensor_tensor(
                        out=xs[:, half * 512:(half + 1) * 512], in0=bank[:, :], in1=xs[:, half * 512:(half + 1) * 512],
                        op=ALU.add), reads=[bk, xk], writes=[xk])
                tr.dma("sp", X_out[tt * 128:(tt + 1) * 128, :], xs[:], reads=[xk], writes=[f"{X_out.name}.{tt}"],
                       semkey=pf + "st" + xk)


def host_consts():
    c = {}
    c["maskT"] = mult_mask_table()
    c["ident"] = np.eye(128, dtype=np.float32).astype(ml_dtypes.bfloat16)
    c["identf"] = np.eye(128, dtype=np.float32)
    return c


def rc16_table(seq_start_own):
    t = np.arange(16)
    rows = []
    for which in range(2):
        vals = []
        for gi in range(4):
            w = 2 << gi
            if which == 0 or seq_start_own:
                cnt = np.minimum(t + 1, w)
            else:
                cnt = np.full(16, w)
            vals.append((1.0 / cnt).astype(np.float32))
        rows.append(np.concatenate(vals))
    return np.stack(rows).astype(np.float32)


def make_in_maps(inputs):
    x = np.ascontiguousarray(inputs["x"], dtype=np.float32)
    B = x.shape[0]
    consts = host_consts()
    shared = dict(consts)
    for name in ("norm_mix", "w_in", "w_out", "norm_ffn", "ffn_wg", "ffn_wu", "ffn_wd", "w_router",
                 "moe_wg", "moe_wu", "moe_wd"):
        shared[name] = np.ascontiguousarray(inputs[name], dtype=np.float32)
    shared["final_norm"] = np.ascontiguousarray(inputs["final_norm"], dtype=np.float32).reshape(1, D)
    shared["w_pool"] = np.ascontiguousarray(np.transpose(inputs["w_pool"], (0, 2, 1, 3)), dtype=np.float32)
    shared["pool_scale"] = np.ascontiguousarray(
        np.transpose(np.asarray(inputs["pool_scale"]).reshape(2, 4, 128), (0, 2, 1)), dtype=np.float32)
    shared["attn_gain"] = np.ascontiguousarray(
        np.transpose(np.asarray(inputs["attn_gain"]).reshape(2, 4, 128), (0, 2, 1)), dtype=np.float32)
    in_maps = []
    for c in range(8):
        b, half = c // 2, c % 2
        m = dict(shared)
        if half == 0:
            xl = np.concatenate([np.zeros((OWN0, D), np.float32), x[b, :OWN0]], axis=0)
        else:
            xl = x[b]
        m["x_loc"] = np.ascontiguousarray(xl)
        m["rc16"] = rc16_table(half == 0)
        m["cflag"] = np.full((128, 1), float(half), np.float32)
        in_maps.append(m)
    return in_maps


_NC_CACHE = {}


def kernel(**inputs):
    if "full" not in _NC_CACHE:
        _NC_CACHE["full"] = build(FULL_PHASES)
    nc = _NC_CACHE["full"]
    in_maps = make_in_maps(inputs)
    res = run_bass_kernel_spmd(nc, in_maps, core_ids=list(range(8)))
    out = np.empty((4, SEQ, D), np.float32)
    for c in range(8):
        b, half = c // 2, c % 2
        out[b, half * OWN0:(half + 1) * OWN0] = res.results[c]["out"]
    return out


def ffn_phase(k, l, X_in, X_out, lo, hi):
    from contextlib import ExitStack
    nc, tr = k.nc, k.tr
    pf = f"f{l}_"
    moe = (l == 1)
    final = (l == 1)
    SGT = 16
    with ExitStack() as es:
        sbt = lambda name, shape, dt: es.enter_context(nc.sbuf_tensor(pf + name, list(shape), dt))
        acc = sbt("acc", [128, SGT, D], F32)
        h2T = sbt("h2T", [128, 8, SGT * 128], BF16)
        gffn = sbt("gffn", [128, D], F32)
        wg_sb = [sbt(f"wg{i}", [128, 8, 512], BF16) for i in range(2)]
        wu_sb = [sbt(f"wu{i}", [128, 8, 512], BF16) for i in range(2)]
        wd_sb = [sbt(f"wd{i}", [128, 4, D], BF16) for i in range(2)]
        actT = sbt("actT", [128, 4, SGT * 128], BF16)
        sgt = [sbt(f"sgt{i}", [128, 512], F32) for i in range(2)]
        hb2 = [sbt(f"hb{i}", [128, D], BF16) for i in range(2)]
        junk = sbt("junk", [128, D], BF16)
        st2 = sbt("st2", [128, 2, 8], F32)
        st = st2[:, 0, :]
        PS, psT = k.ps, k.psT
        tr.dma("sp", gffn[:], k.norm_ffn[l:l + 1, :].partition_broadcast(128), writes=["gffn"], semkey="fc")
        if moe:
            gate = sbt("gate", [128, SGT, NE], F32)
            hf2 = [sbt(f"hf{i}", [128, D], F32) for i in range(2)]
            hT32 = sbt("hT32", [128, 8, 128], F32)
            wr = sbt("wr", [128, 8, NE], F32)
            gt = sbt("gt", [128, 8, NE], F32)
            gfin = sbt("gfin", [128, D], F32)
            tr.dma("sp", wr[:], k.w_router[0].rearrange("(c p) e -> p c e", p=128), writes=["wr"], semkey="fc")
            tr.dma("sp", gfin[:], k.final_norm[0:1, :].partition_broadcast(128), writes=["gfin"], semkey="fc")
        blocks = []
        if moe:
            for e_ in range(NE):
                for f0 in range(0, D_FFE, 512):
                    blocks.append((k.moe_wg[0, e_], k.moe_wu[0, e_], k.moe_wd[0, e_], f0, 4, e_))
        else:
            for f0 in range(0, D_FF, 512):
                blocks.append((k.ffn_wg[0], k.ffn_wu[0], k.ffn_wd[0], f0, min(4, (D_FF - f0) // 128), None))
        nb = len(blocks)
        bctr = [0]

        def load_block(bi, par):
            wg_ap, wu_ap, wd_ap, f0, nf, _ = blocks[bi]
            fw = nf * 128
            for (dst, src, key) in ((wg_sb[par], wg_ap, f"wg{par}"), (wu_sb[par], wu_ap, f"wu{par}")):
                for c0 in (0, 4):
                    tr.dma("pool", dst[:, c0:c0 + 4, 0:fw],
                           src[c0 * 128:(c0 + 4) * 128, f0:f0 + fw].rearrange("(c p) f -> p c f", p=128),
                           writes=[key], semkey=pf + key)
            tr.dma("pool", wd_sb[par][:, 0:nf, :], wd_ap[f0:f0 + fw, :].rearrange("(c p) n -> p c n", p=128),
                   writes=[f"wd{par}"], semkey=pf + f"wd{par}")

        for sg0 in range(lo // 128, hi // 128, SGT):
            load_block(0, bctr[0] % 2)
            def stage1(t):
                tt = sg0 + t
                ak = f"acc{t}"
                par = t % 2
                kp = f"p{par}"
                stp = st2[:, par, :]
                tr.dma("sp", acc[:, t, :], X_in[tt * 128:(tt + 1) * 128, :], reads=[f"{X_in.name}.{tt}"], writes=[ak],
                       semkey=pf + f"acc{t % 4}")
                tr.op("dve", lambda e: e.memset(stp[:, 0:1], 0.0), writes=[kp + "st0"])
                tr.op("act", lambda e: e.activation(out=junk[:], in_=acc[:, t, :], func=AF.Square,
                                                    accum_out=stp[:, 0:1]), reads=[ak], writes=["junk", kp + "st0"])
                rms_rstd(tr, stp, D, kp)
                tr.op("dve", lambda e: e.scalar_tensor_tensor(
                    out=hb2[par][:], in0=acc[:, t, :], scalar=stp[:, 2:3], in1=gffn[:], op0=ALU.mult, op1=ALU.mult),
                    reads=[ak, kp + "st2", "gffn"], writes=[f"hb{par}"])
                if moe:
                    tr.op("dve", lambda e: e.scalar_tensor_tensor(
                        out=hf2[par][:], in0=acc[:, t, :], scalar=stp[:, 2:3], in1=gffn[:], op0=ALU.mult, op1=ALU.mult),
                        reads=[ak, kp + "st2", "gffn"], writes=[f"hf{par}"])

            def stage2(t):
                par = t % 2
                kp = f"p{par}"
                stp = st2[:, par, :]
                hbp, hbk = hb2[par], f"hb{par}"
                for c in range(8):
                    tr.op("pe", lambda e, c=c: e.transpose(out=psT[:, c, :], in_=hbp[:, c * 128:(c + 1) * 128],
                                                           identity=k.idb[:]), reads=[hbk, "idb"], writes=["psT"])
                tr.op("act", lambda e: e.copy(out=h2T[:, :, t * 128:(t + 1) * 128], in_=psT[:, :, :]),
                      reads=["psT"], writes=[f"h2T{t // 4}"])
                if moe:
                    hfp, hfk = hf2[par], f"hf{par}"
                    for c in range(8):
                        bank, bk = PS[5 + c // 4], f"ps{5 + c // 4}"
                        tr.op("pe", lambda e, c=c, bank=bank: e.transpose(
                            out=bank[:, (c % 4) * 128:(c % 4 + 1) * 128], in_=hfp[:, c * 128:(c + 1) * 128],
                            identity=k.idf[:]), reads=[hfk, "idf"], writes=[bk])
                    tr.op("act", lambda e: e.copy(out=hT32[:, 0:4, :], in_=PS[5][:, :].rearrange("p (c t) -> p c t", t=128)),
                          reads=["ps5"], writes=["hT32"])
                    tr.op("dve", lambda e: e.tensor_copy(out=hT32[:, 4:8, :], in_=PS[6][:, :].rearrange("p (c t) -> p c t", t=128)),
                          reads=["ps6"], writes=["hT32"])
                    for c in range(8):
                        tr.op("pe", lambda e, c=c: e.matmul(PS[4][:, 0:NE], lhsT=hT32[:, c, :], rhs=wr[:, c, :],
                                                            start=(c == 0), stop=(c == 7)),
                              reads=["hT32", "wr"], writes=["ps4"])
                    lg, eq1, lg2, sel, ex = gt[:, 0, :], gt[:, 1, :], gt[:, 2, :], gt[:, 3, :], gt[:, 4, :]
                    S = lambda i: stp[:, i:i + 1]
                    tr.op("dve", lambda e: e.tensor_copy(out=lg, in_=PS[4][:, 0:NE]), reads=["ps4"], writes=["lg"])
                    tr.op("dve", lambda e: e.reduce_max(out=S(3), in_=lg, axis=AX.X), reads=["lg"], writes=[kp + "st3"])
                    tr.op("dve", lambda e: e.tensor_scalar(out=eq1, in0=lg, scalar1=S(3), scalar2=None,
                                                            op0=ALU.is_equal), reads=["lg", kp + "st3"], writes=["eq1"])
                    tr.op("dve", lambda e: e.scalar_tensor_tensor(out=lg2, in0=eq1, scalar=-1e30, in1=lg,
                                                                   op0=ALU.mult, op1=ALU.add),
                          reads=["eq1", "lg"], writes=["lg2"])
                    tr.op("dve", lambda e: e.reduce_max(out=S(4), in_=lg2, axis=AX.X), reads=["lg2"], writes=[kp + "st4"])
                    tr.op("dve", lambda e: e.tensor_scalar(out=sel, in0=lg, scalar1=S(4), scalar2=None,
                                                            op0=ALU.is_ge), reads=["lg", kp + "st4"], writes=["sel"])
                    tr.op("dve", lambda e: e.tensor_scalar(out=S(5), in0=S(3), scalar1=-1.0, scalar2=None,
                                                            op0=ALU.mult), reads=[kp + "st3"], writes=[kp + "st5"])
                    tr.op("act", lambda e: e.activation(out=ex, in_=lg, func=AF.Exp, bias=S(5), scale=1.0),
                          reads=["lg", kp + "st5"], writes=["ex"])
                    tr.op("dve", lambda e: e.tensor_tensor(out=ex, in0=ex, in1=sel, op=ALU.mult),
                          reads=["ex", "sel"], writes=["ex"])
                    tr.op("dve", lambda e: e.reduce_sum(out=S(6), in_=ex, axis=AX.X), reads=["ex"], writes=[kp + "st6"])
                    tr.op("dve", lambda e: e.reciprocal(out=S(7), in_=S(6)), reads=[kp + "st6"], writes=[kp + "st7"])
                    tr.op("dve", lambda e: e.tensor_scalar(out=gate[:, t, :], in0=ex, scalar1=S(7), scalar2=None,
                                                            op0=ALU.mult), reads=["ex", kp + "st7"], writes=[f"gate{t}"])

            for t in range(SGT):
                stage1(t)
                if t >= 1:
                    stage2(t - 1)
            stage2(SGT - 1)
            hkeys = [f"h2T{i}" for i in range(4)]
            it = 0
            for bi in range(nb):
                par = bctr[0] % 2
                if bi + 1 < nb:
                    load_block(bi + 1, (bctr[0] + 1) % 2)
                _, _, _, f0, nf, ex_i = blocks[bi]
                for fc in range(nf):
                    for gq in range(4):
                        Gb, Ub = PS[2 * (it % 2)], PS[2 * (it % 2) + 1]
                        gk, uk = f"ps{2 * (it % 2)}", f"ps{2 * (it % 2) + 1}"
                        for (bank, bkey, wsb, wkey) in ((Gb, gk, wg_sb[par], f"wg{par}"), (Ub, uk, wu_sb[par], f"wu{par}")):
                            for c in range(8):
                                tr.op("pe", lambda e, c=c, bank=bank, wsb=wsb, fc=fc, gq=gq: e.matmul(
                                    bank[:, :], lhsT=wsb[:, c, fc * 128:(fc + 1) * 128], rhs=h2T[:, c, gq * 512:(gq + 1) * 512],
                                    start=(c == 0), stop=(c == 7)), reads=[wkey, f"h2T{gq}"], writes=[bkey])
                        sg_, sk = sgt[it % 2], f"sgt{it % 2}"
                        tr.op("act", lambda e, Gb=Gb, sg_=sg_: e.activation(out=sg_[:, :], in_=Gb[:, :], func=AF.Silu),
                              reads=[gk], writes=[sk])
                        tr.op("dve", lambda e, Ub=Ub, sg_=sg_, fc=fc, gq=gq: e.tensor_tensor(
                            out=actT[:, fc, gq * 512:(gq + 1) * 512], in0=Ub[:, :], in1=sg_[:, :], op=ALU.mult),
                            reads=[uk, sk], writes=[f"actT{gq}"])
                        it += 1
                for t in range(SGT):
                    for half in range(2):
                        bank, bk = PS[4 + (t * 2 + half) % NB2], f"ps{4 + (t * 2 + half) % NB2}"
                        for fc in range(nf):
                            tr.op("pe", lambda e, fc=fc, t=t, half=half, bank=bank: e.matmul(
                                bank[:, :], lhsT=actT[:, fc, t * 128:(t + 1) * 128],
                                rhs=wd_sb[par][:, fc, half * 512:(half + 1) * 512], start=(fc == 0), stop=(fc == nf - 1)),
                                reads=[f"actT{t // 4}", f"wd{par}"], writes=[bk])
                        asl = acc[:, t, half * 512:(half + 1) * 512]
                        if moe:
                            tr.op("dve", lambda e, bank=bank, asl=asl, t=t, ex_i=ex_i: e.scalar_tensor_tensor(
                                out=asl, in0=bank[:, :], scalar=gate[:, t, ex_i:ex_i + 1], in1=asl,
                                op0=ALU.mult, op1=ALU.add), reads=[bk, f"acc{t}", f"gate{t}"], writes=[f"acc{t}"])
                        else:
                            tr.op("dve", lambda e, bank=bank, asl=asl: e.tensor_tensor(out=asl, in0=bank[:, :], in1=asl,
                                                                                      op=ALU.add),
                                  reads=[bk, f"acc{t}"], writes=[f"acc{t}"])
                bctr[0] += 1
            for t in range(SGT):
                tt = sg0 + t
                ak = f"acc{t}"
                if final:
                    tr.op("dve", lambda e: e.memset(st[:, 0:1], 0.0), writes=["p0st0"])
                    tr.op("act", lambda e, t=t: e.activation(out=junk[:], in_=acc[:, t, :], func=AF.Square,
                                                             accum_out=st[:, 0:1]), reads=[ak], writes=["junk", "p0st0"])
                    rms_rstd(tr, st, D, "p0")
                    tr.op("dve", lambda e, t=t: e.scalar_tensor_tensor(
                        out=acc[:, t, :], in0=acc[:, t, :], scalar=st[:, 2:3], in1=gfin[:], op0=ALU.mult, op1=ALU.mult),
                        reads=[ak, "p0st2", "gfin"], writes=[ak])
                    ro = tt - lo // 128
                else:
                    ro = tt
                tr.dma("sp", X_out[ro * 128:(ro + 1) * 128, :], acc[:, t, :], reads=[ak], writes=[f"{X_out.name}.{ro}"],
                       semkey=pf + f"st{t % 4}")
```

```python
import numpy as np
import ml_dtypes
import concourse.bass as bass
import concourse.mybir as mybir
from concourse.bass_utils import run_bass_kernel_spmd

F32 = mybir.dt.float32
BF16 = mybir.dt.bfloat16
ALU = mybir.AluOpType
AF = mybir.ActivationFunctionType
AX = mybir.AxisListType

D = 1024
SEQ = 4096
NT = 4096
OWN0 = 2048
G = 512
NG = NT // G
D_IN = 2048
D_FF = 2816
NE = 8
D_FFE = 3584
EPS = 1e-6
MASK_LO = 3
MASK_N = 23
NWARM0 = 8
NWARM1 = 1
NB2 = 3


class Tr:
    EPOCH = 12000

    def __init__(self, nc):
        self.nc = nc
        self.eng = {"pe": nc.tensor, "act": nc.scalar, "dve": nc.vector, "pool": nc.gpsimd, "sp": nc.sync}
        self.cnt = {e: 0 for e in self.eng}
        self.nsem = 0
        self.sem = {e: self.new_sem(e) for e in self.eng}
        self.own = {e: {id(self.sem[e])} for e in self.eng}
        self.semobj = {}
        self.lw = {}
        self.rd = {}
        self.waited = {e: {} for e in self.eng}
        self.dsem = {}
        self.dcnt = {}

    def new_sem(self, name):
        self.nsem += 1
        return self.nc.alloc_semaphore(f"s_{name}_{self.nsem}")

    def _deps(self, e, reads, writes):
        deps = []
        for k in list(reads) + list(writes):
            ev = self.lw.get(k)
            if ev is not None:
                deps.append((ev, True))
        for k in writes:
            for ev in self.rd.get(k, {}).values():
                deps.append((ev, False))
        out = {}
        for (ev, is_w) in deps:
            sem, val, src = ev
            if src == e:
                if e in ("pe", "sp"):
                    continue
                if not is_w:
                    continue
            key = id(sem)
            if self.waited[e].get(key, 0) >= val:
                continue
            if key not in out or out[key][1] < val:
                out[key] = (sem, val)
        for key, (sem, val) in out.items():
            self.eng[e].wait_ge(sem, val)
            self.waited[e][key] = val

    def _record(self, ev, reads, writes):
        for k in writes:
            self.lw[k] = ev
            self.rd[k] = {}
        for k in reads:
            self.rd.setdefault(k, {})[id(ev[0])] = ev

    def op(self, e, fn, reads=(), writes=()):
        self._deps(e, reads, writes)
        ins = fn(self.eng[e])
        self.cnt[e] += 1
        ins.then_inc(self.sem[e], 1)
        ev = (self.sem[e], self.cnt[e], e)
        self._record(ev, reads, writes)
        if self.cnt[e] >= self.EPOCH:
            self.sem[e] = self.new_sem(e)
            self.cnt[e] = 0
        return ev

    def dma(self, q, out, in_, reads=(), writes=(), semkey=None):
        self._deps(q, reads, writes)
        if semkey not in self.dsem:
            self.dsem[semkey] = self.new_sem("d")
            self.dcnt[semkey] = 0
        sem = self.dsem[semkey]
        self.eng[q].dma_start(out=out, in_=in_).then_inc(sem, 16)
        self.dcnt[semkey] += 16
        ev = (sem, self.dcnt[semkey], "dma")
        self._record(ev, reads, writes)
        return ev

    def barrier(self):
        evs = [(self.sem[e], self.cnt[e]) for e in self.eng if self.cnt[e] > 0]
        evs += [(self.dsem[k], self.dcnt[k]) for k in self.dsem if self.dcnt[k] > 0]
        for e in self.eng:
            for sem, val in evs:
                if sem is self.sem[e]:
                    continue
                if self.waited[e].get(id(sem), 0) >= val:
                    continue
                self.eng[e].wait_ge(sem, val)
                self.waited[e][id(sem)] = val

    def wait_all_dma(self, q, keys):
        for k in keys:
            if k in self.dsem and self.dcnt[k] > 0:
                self.eng[q].wait_ge(self.dsem[k], self.dcnt[k])


def mult_mask_table():
    kk = np.arange(128)[:, None]
    qq = np.arange(128)[None, :]
    tab = np.zeros((128, MASK_N * 128), np.float32)
    for di in range(MASK_N):
        delta = di - MASK_LO
        off = delta * 128 + qq - kk
        m = np.zeros_like(off, dtype=np.float32)
        m += ((off >= 0) & (off <= 128)).astype(np.float32)
        m += ((off >= 0) & (off <= 512) & (off % 4 == 0)).astype(np.float32)
        m += ((off >= 0) & (off <= 2048) & (off % 16 == 0)).astype(np.float32)
        tab[:, di * 128:(di + 1) * 128] = m
    return tab.astype(ml_dtypes.bfloat16)


class K:
    pass


def build(phases=("m0", "f0", "m1", "f1"), debug=False):
    nc = bass.Bass("TRN2", target_bir_lowering=False)
    k = K()
    k.nc = nc
    k.tr = Tr(nc)
    tr = k.tr
    dt_in = lambda name, shape, dt=F32: nc.dram_tensor(name, list(shape), dt, kind="ExternalInput").ap()
    k.x_loc = dt_in("x_loc", [NT, D])
    k.norm_mix = dt_in("norm_mix", [2, D])
    k.w_in = dt_in("w_in", [2, D, D_IN])
    k.w_pool = dt_in("w_pool", [2, 4, 128, 128])
    k.pool_scale = dt_in("pool_scale", [2, 512])
    k.attn_gain = dt_in("attn_gain", [2, 512])
    k.w_out = dt_in("w_out", [2, D, D])
    k.norm_ffn = dt_in("norm_ffn", [2, D])
    k.ffn_wg = dt_in("ffn_wg", [1, D, D_FF])
    k.ffn_wu = dt_in("ffn_wu", [1, D, D_FF])
    k.ffn_wd = dt_in("ffn_wd", [1, D_FF, D])
    k.w_router = dt_in("w_router", [1, D, NE])
    k.moe_wg = dt_in("moe_wg", [1, NE, D, D_FFE])
    k.moe_wu = dt_in("moe_wu", [1, NE, D, D_FFE])
    k.moe_wd = dt_in("moe_wd", [1, NE, D_FFE, D])
    k.final_norm = dt_in("final_norm", [1, D])
    k.maskT = dt_in("maskT", [128, MASK_N * 128], BF16)
    k.ident = dt_in("ident", [128, 128], BF16)
    k.identf = dt_in("identf", [128, 128], F32)
    k.rc16 = dt_in("rc16", [2, 64])
    k.cflag = dt_in("cflag", [128, 1])
    k.out = nc.dram_tensor("out", [NT - OWN0, D], F32, kind="ExternalOutput").ap()
    skind = "ExternalOutput" if debug else "Internal"
    k.xa = nc.dram_tensor("xa", [NT, D], F32, kind=skind).ap()
    k.xb = nc.dram_tensor("xb", [NT, D], F32, kind=skind).ap()
    k.xc = nc.dram_tensor("xc", [NT, D], F32, kind=skind).ap()
    bufs = {"x_loc": k.x_loc, "xa": k.xa, "xb": k.xb, "xc": k.xc, "out": k.out}
    sb = lambda name, shape, dt: nc.alloc_sbuf_tensor(name, list(shape), dt)
    ps = lambda name, shape, dt: nc.alloc_psum_tensor(name, list(shape), dt)
    k.psall = ps("psall", [128, 7, 512], F32)
    k.ps = [k.psall[:, i, :] for i in range(7)]
    k.psT = ps("psT", [128, 8, 128], BF16)
    k.mt = sb("mt", [128, MASK_N * 128], BF16)
    k.idb = sb("idb", [128, 128], BF16)
    k.idf = sb("idf", [128, 128], F32)
    k.ones = sb("ones", [128, 128], BF16)
    k.cfl = sb("cfl", [128, 1], F32)
    k.rc = sb("rc", [128, 2, 64], F32)
    tr.dma("sp", k.mt[:], k.maskT, writes=["mt"], semkey="const")
    tr.dma("sp", k.idb[:], k.ident, writes=["idb"], semkey="const")
    tr.dma("sp", k.idf[:], k.identf, writes=["idf"], semkey="const")
    tr.dma("sp", k.cfl[:], k.cflag, writes=["cfl"], semkey="const")
    tr.dma("sp", k.rc[:].rearrange("p a b -> p (a b)"),
           k.rc16.rearrange("a b -> (a b)").partition_broadcast(128), writes=["rc"], semkey="const")
    tr.op("dve", lambda e: e.memset(k.ones[:], 1.0), writes=["ones"])
    for (kind, l, src, dst, lo, hi) in phases:
        if kind == "m":
            mixer_phase(k, l, bufs[src], bufs[dst], lo, hi)
        else:
            ffn_phase(k, l, bufs[src], bufs[dst], lo, hi)
        tr.barrier()
    tr.wait_all_dma("sp", list(tr.dsem.keys()))
    return nc


FULL_PHASES = (("m", 0, "x_loc", "xa", 0, NG), ("f", 0, "xa", "xb", 0, NT),
               ("m", 1, "xb", "xc", NG // 2, NG), ("f", 1, "xc", "out", OWN0, NT))


def _ap2(base_ap, stride):
    return bass.AP(base_ap.tensor, base_ap.offset, [list(base_ap.ap[0]), [stride, 2], [1, 64]])


def rms_rstd(tr, st, n, kp=""):
    tr.op("act", lambda e: e.activation(out=st[:, 1:2], in_=st[:, 0:1], func=AF.Ln, scale=1.0 / n, bias=EPS),
          reads=[kp + "st0"], writes=[kp + "st1"])
    tr.op("act", lambda e: e.activation(out=st[:, 2:3], in_=st[:, 1:2], func=AF.Exp, scale=-0.5),
          reads=[kp + "st1"], writes=[kp + "st2"])


def rstd_tile(tr, rstd, psbank, pskey, n):
    tr.op("act", lambda e: e.activation(out=rstd[:, :], in_=psbank[:, :], func=AF.Ln, scale=1.0 / n, bias=EPS),
          reads=[pskey], writes=["rstd"])
    tr.op("act", lambda e: e.activation(out=rstd[:, :], in_=rstd[:, :], func=AF.Exp, scale=-0.5),
          reads=["rstd"], writes=["rstd"])


def mixer_phase(k, l, X_in, X_out, g_lo, g_hi):
    from contextlib import ExitStack
    nc, tr = k.nc, k.tr
    pf = f"m{l}_"
    with ExitStack() as es:
        sbt = lambda name, shape, dt: es.enter_context(nc.sbuf_tensor(pf + name, list(shape), dt))
        w_in = sbt("w_in", [128, 8, D_IN], BF16)
        w_out = sbt("w_out", [128, 8, D], BF16)
        w_pool = sbt("w_pool", [128, 4, 128], BF16)
        gmix = sbt("gmix", [128, D], F32)
        pscale = sbt("pscale", [128, 4], F32)
        again = sbt("again", [128, 4], F32)
        kT = sbt("kT", [128, 4, 5 * G], BF16)
        vr = sbt("vr", [128, 20, 8, 128], BF16)
        xt = [sbt(f"xt{i}", [128, D], F32) for i in range(2)]
        hb = [sbt(f"hb{i}", [128, D], BF16) for i in range(2)]
        st2 = sbt("st2", [128, 2, 8], F32)
        junk = sbt("junk", [128, D], BF16)
        hT = sbt("hT", [128, 8, G], BF16)
        uT = sbt("uT", [128, 4, 16 + G], F32)
        sA = sbt("sA", [128, 16 + G], F32)
        sB = sbt("sB", [128, 16 + G], F32)
        tmp16 = sbt("tmp16", [128, 16], F32)
        dT = sbt("dT", [128, 4, G], BF16)
        qT = sbt("qT", [128, 4, G], BF16)
        zo = sbt("zo", [128, 4, G], F32)
        sq = sbt("sq", [128, 4, G], BF16)
        rstd = sbt("rstd", [128, G], F32)
        yT = sbt("yT", [128, 8, G], BF16)
        NPB = 4
        pT = [sbt(f"pT{i}", [128, 2, G], BF16) for i in range(NPB)]
        S2 = [k.psall[:, 2:4, :], k.psall[:, 0:2, :]]
        S2K = [["ps2", "ps3"], ["ps0", "ps1"]]
        SKEW = 2
        rz = sbt("rz", [128, G], F32)
        xr = [sbt(f"xr{i}", [128, D], F32) for i in range(2)]
        PS = k.ps
        psT = k.psT

        for c in range(8):
            tr.dma("pool", w_in[:, c, :], k.w_in[l, c * 128:(c + 1) * 128, :], writes=["w_in"], semkey="mw")
        for c in range(8):
            tr.dma("pool", w_out[:, c, :], k.w_out[l, c * 128:(c + 1) * 128, :], writes=["w_out"], semkey="mw")
        tr.dma("pool", w_pool[:], k.w_pool[l], writes=["w_pool"], semkey="mw")
        tr.dma("sp", gmix[:], k.norm_mix[l:l + 1, :].partition_broadcast(128), writes=["gmix"], semkey="mc")
        tr.dma("sp", pscale[:], k.pool_scale[l], writes=["pscale"], semkey="mc")
        tr.dma("sp", again[:], k.attn_gain[l], writes=["again"], semkey="mc")
        vkeys = [f"vr{i}" for i in range(20)]
        tr.op("pool", lambda e: e.memset(vr[:, :, :, :], 1.0), writes=vkeys)
        tr.op("dve", lambda e: e.memset(uT[:, :, 0:16], 0.0), writes=[f"uT{j}" for j in range(4)])

        def stage1(gg, t):
            tt = 4 * gg + t
            xs, hs = xt[tt % 2], hb[tt % 2]
            xk, hk = f"xt{tt % 2}", f"hb{tt % 2}"
            kp = f"p{tt % 2}"
            stp = st2[:, tt % 2, :]
            tr.dma("sp", xs[:], X_in[tt * 128:(tt + 1) * 128, :], reads=[f"{X_in.name}.{tt}"], writes=[xk],
                   semkey=pf + xk)
            tr.op("dve", lambda e: e.memset(stp[:, 0:1], 0.0), writes=[kp + "st0"])
            tr.op("act", lambda e: e.activation(out=junk[:], in_=xs[:], func=AF.Square, accum_out=stp[:, 0:1]),
                  reads=[xk], writes=["junk", kp + "st0"])
            rms_rstd(tr, stp, D, kp)
            tr.op("dve", lambda e: e.scalar_tensor_tensor(
                out=hs[:], in0=xs[:], scalar=stp[:, 2:3], in1=gmix[:], op0=ALU.mult, op1=ALU.mult),
                reads=[xk, kp + "st2", "gmix"], writes=[hk])

        def stage2(gg, t):
            tt = 4 * gg + t
            hs, hk = hb[tt % 2], f"hb{tt % 2}"
            for c in range(8):
                tr.op("pe", lambda e, c=c: e.transpose(out=psT[:, c, :], in_=hs[:, c * 128:(c + 1) * 128],
                                                       identity=k.idb[:]),
                      reads=[hk, "idb"], writes=["psT"])
            tr.op("act", lambda e: e.copy(out=hT[:, :, t * 128:(t + 1) * 128], in_=psT[:, :, :]),
                  reads=["psT"], writes=[f"hT{t}"])

        def stepA_block(gg):
            for t in range(4):
                stage1(gg, t)
                if t >= 1:
                    stage2(gg, t - 1)
            stage2(gg, 3)

        stepA_block(0)
        for g in range(NG):
            active = g_lo <= g < g_hi
            hkeys = [f"hT{t}" for t in range(4)]
            def pool_windows():
                for gi in range(4):
                    w = 2 << gi
                    a = uT[:, gi, :]
                    chain = [(sA, "sA", 1, 1)]
                    if gi >= 1:
                        chain.append((sB, "sB", 3, 2))
                    if gi >= 2:
                        chain.append((sA, "sA", 7, 4))
                    if gi >= 3:
                        chain.append((sB, "sB", 15, 8))
                    src, srck = a, f"uT{gi}"
                    for (dst, dk, i0, sh) in chain:
                        tr.op("dve", lambda e, dst=dst, src=src, i0=i0, sh=sh: e.tensor_tensor(
                            out=dst[:, i0:16 + G], in0=src[:, i0:16 + G], in1=src[:, i0 - sh:16 + G - sh], op=ALU.add),
                            reads=[srck], writes=[dk])
                        src, srck = dst, dk
                    tr.op("dve", lambda e, src=src, a=a, gi=gi, w=w: e.scalar_tensor_tensor(
                        out=dT[:, gi, :], in0=src[:, 16:16 + G], scalar=1.0 / w, in1=a[:, 16:16 + G],
                        op0=ALU.mult, op1=ALU.subtract), reads=[srck, f"uT{gi}"], writes=[f"dT{gi}"])
                    if g == 0 or g == NG // 2:
                        which = 0 if g == 0 else 1
                        tr.op("dve", lambda e, src=src, gi=gi, which=which: e.tensor_tensor(
                            out=tmp16[:, :], in0=src[:, 16:32], in1=k.rc[:, which, gi * 16:(gi + 1) * 16], op=ALU.mult),
                            reads=[srck, "rc"], writes=["tmp16"])
                        tr.op("dve", lambda e, a=a, gi=gi: e.tensor_tensor(
                            out=dT[:, gi, 0:16], in0=tmp16[:, :], in1=a[:, 16:32], op=ALU.subtract),
                            reads=["tmp16", f"uT{gi}"], writes=[f"dT{gi}"])

            for j in range(12):
                if 4 <= j < 8 and not active:
                    continue
                bank, bk = PS[j % 2], f"ps{j % 2}"
                for c in range(8):
                    tr.op("pe", lambda e, c=c, j=j, bank=bank: e.matmul(
                        bank[:, :], lhsT=w_in[:, c, j * 128:(j + 1) * 128], rhs=hT[:, c, :],
                        start=(c == 0), stop=(c == 7)), reads=["w_in"] + hkeys, writes=[bk])
                if j < 4:
                    tr.op("act", lambda e, j=j, bank=bank: e.copy(out=uT[:, j, 16:16 + G], in_=bank[:, :]),
                          reads=[bk], writes=[f"uT{j}"])
                    if j == 3 and active:
                        pool_windows()
                elif j < 8:
                    tr.op("act", lambda e, j=j, bank=bank: e.activation(out=qT[:, j - 4, :], in_=bank[:, :],
                                                                       func=AF.Copy, scale=0.125),
                          reads=[bk], writes=[f"qT{j - 4}"])
                else:
                    s0 = (g % 5) * G
                    tr.op("dve", lambda e, j=j, bank=bank, s0=s0: e.tensor_copy(out=kT[:, j - 8, s0:s0 + G],
                                                                               in_=bank[:, :]),
                          reads=[bk], writes=[f"kT{j - 8}_{g % 5}"])
            for t in range(4):
                tt = 4 * g + t
                bank, bk = PS[t % 2], f"ps{t % 2}"
                for c in range(8):
                    tr.op("pe", lambda e, c=c, t=t, bank=bank: e.matmul(
                        bank[:, :], lhsT=hT[:, c, t * 128:(t + 1) * 128], rhs=w_in[:, c, 1536:2048],
                        start=(c == 0), stop=(c == 7)), reads=["w_in", f"hT{t}"], writes=[bk])
                bv = bank[:, :].rearrange("p (h d) -> p h d", d=64)
                tr.op("dve", lambda e, tt=tt, bv=bv: e.tensor_copy(out=vr[:, tt % 20, 0:8:2, 0:64], in_=bv[:, 0:8:2, :]),
                      reads=[bk], writes=[f"vr{tt % 20}"])
                tr.op("dve", lambda e, tt=tt, bv=bv: e.tensor_copy(out=vr[:, tt % 20, 1:8:2, 64:128], in_=bv[:, 1:8:2, :]),
                      reads=[bk], writes=[f"vr{tt % 20}"])
                if tt >= 20:
                    tr.op("dve", lambda e, tt=tt: e.memset(vr[:, tt % 20, 0:8:2, 64:128], 1.0), writes=[f"vr{tt % 20}"])
                    tr.op("dve", lambda e, tt=tt: e.memset(vr[:, tt % 20, 1:8:2, 0:64], 1.0), writes=[f"vr{tt % 20}"])
            if active:
                for gi in range(4):
                    bank, bk = PS[gi % 2], f"ps{gi % 2}"
                    tr.op("pe", lambda e, gi=gi, bank=bank: e.matmul(bank[:, :], lhsT=w_pool[:, gi, :], rhs=dT[:, gi, :],
                                                                     start=True, stop=True),
                          reads=["w_pool", f"dT{gi}"], writes=[bk])
                    tr.op("act", lambda e, gi=gi, bank=bank: e.copy(out=zo[:, gi, :], in_=bank[:, :]),
                          reads=[bk], writes=[f"zo{gi}"])
                    tr.op("act", lambda e, gi=gi, bank=bank: e.activation(out=sq[:, gi, :], in_=bank[:, :],
                                                                         func=AF.Square),
                          reads=[bk], writes=[f"sq{gi}"])

                def pool_norm():
                    for gi in range(4):
                        tr.op("pe", lambda e, gi=gi: e.matmul(PS[4][:, :], lhsT=k.ones[:, :], rhs=sq[:, gi, :],
                                                              start=(gi == 0), stop=(gi == 3)),
                              reads=["ones", f"sq{gi}"], writes=["ps4"])
                    rstd_tile(tr, rstd, PS[4], "ps4", 512)
                    for gi in range(4):
                        tr.op("dve", lambda e, gi=gi: e.scalar_tensor_tensor(
                            out=yT[:, gi, :], in0=zo[:, gi, :], scalar=pscale[:, gi:gi + 1], in1=rstd[:, :],
                            op0=ALU.mult, op1=ALU.mult), reads=[f"zo{gi}", "pscale", "rstd"], writes=[f"yT{gi}"])
            for j in range(4):
                tr.op("pool", lambda e, j=j: e.tensor_copy(out=uT[:, j, 0:16], in_=uT[:, j, G:G + 16]),
                      reads=[f"uT{j}"], writes=[f"uT{j}"])
            if not active:
                if g + 1 < NG:
                    stepA_block(g + 1)
                continue
            for t in (0, 1):
                tt = 4 * g + t
                tr.dma("sp", xr[t][:], X_in[tt * 128:(tt + 1) * 128, :], reads=[f"{X_in.name}.{tt}"], writes=[f"xr{t}"],
                       semkey=pf + f"xr{t}")
            kt0 = max(0, 4 * g - 16)
            if g == NG // 2:
                for kt in range(0, OWN0 // 128):
                    tr.op("dve", lambda e, kt=kt: e.tensor_scalar(
                        out=vr[:, kt % 20, :, :], in0=vr[:, kt % 20, :, :], scalar1=k.cfl[:, 0:1], scalar2=None,
                        op0=ALU.mult), reads=[f"vr{kt % 20}", "cfl"], writes=[f"vr{kt % 20}"])
            for j in range(4):
                items = []
                for kt in range(kt0, 4 * g + 4, 2):
                    for hh in range(2):
                        items.append((kt + 1, kt, hh))
                XB = [PS[5], PS[6]]
                nit = len(items)
                first = [True, True]

                def qk(i):
                    kth, ktl, hh = items[i]
                    S, sks = S2[i % 2], S2K[i % 2]
                    p0 = 64 * hh
                    for half, kt in enumerate((kth, ktl)):
                        kg = kt // 4
                        ko = (kg % 5) * G + (kt % 4) * 128
                        tr.op("pe", lambda e, half=half, ko=ko: e.matmul(
                            S[:, half, :], lhsT=kT[p0:p0 + 64, j, ko:ko + 128], rhs=qT[p0:p0 + 64, j, :],
                            start=True, stop=True), reads=[f"kT{j}_{kg % 5}", f"qT{j}"], writes=[sks[half]])
                    pk = f"pT{i % NPB}"
                    pb = pT[i % NPB]
                    tr.op("act", lambda e: e.activation(out=pb[:, :, :], in_=S[:, :, :], func=AF.Exp),
                          reads=sks, writes=[pk])
                    di0 = (4 * g - kth) + MASK_LO
                    mb = k.mt[:, di0 * 128:di0 * 128 + G]
                    msk = bass.AP(mb.tensor, mb.offset, [list(mb.ap[0]), [128, 2], [1, G]])
                    tr.op("dve", lambda e: e.tensor_tensor(out=pb[:, :, :], in0=pb[:, :, :], in1=msk, op=ALU.mult),
                          reads=[pk, "mt"], writes=[pk])

                def pv(i):
                    kth, ktl, hh = items[i]
                    h = 2 * j + hh
                    X = XB[hh]
                    for half, kt in enumerate((kth, ktl)):
                        slot = kt % 20
                        st_ = first[hh]
                        first[hh] = False
                        tr.op("pe", lambda e, half=half, slot=slot, st_=st_, kt=kt: e.matmul(
                            X[:, :], lhsT=vr[:, slot, h, :], rhs=pT[i % NPB][:, half, :],
                            start=st_, stop=(kt == 4 * g + 3), skip_group_check=True),
                            reads=[f"vr{slot}", f"pT{i % NPB}"], writes=[f"ps{5 + hh}"])

                def warm(n):
                    for _ in range(n):
                        tr.op("pe", lambda e: e.matmul(PS[4][:, :], lhsT=k.ones[:, :], rhs=k.mt[:, 0:G],
                                                       start=True, stop=True), reads=["ones", "mt"], writes=["ps4"])
                if NWARM0:
                    warm(NWARM0)
                for i in range(nit + SKEW):
                    if i < nit:
                        qk(i)
                        if NWARM1:
                            warm(NWARM1)
                    if i >= SKEW:
                        pv(i - SKEW)
                if g + 1 < NG:
                    stage1(g + 1, j)
                    if j >= 1:
                        stage2(g + 1, j - 1)
                if j == 0:
                    pool_norm()
                tr.op("act", lambda e: e.activation(out=rz[64:128, :], in_=PS[5][64:128, :], func=AF.Ln),
                      reads=["ps5"], writes=["rzA"])
                tr.op("act", lambda e: e.activation(out=rz[64:128, :], in_=rz[64:128, :], func=AF.Exp, scale=-1.0),
                      reads=["rzA"], writes=["rzA"])
                tr.op("dve", lambda e: e.tensor_tensor(out=zo[0:64, j, :], in0=PS[5][0:64, :], in1=rz[64:128, :],
                                                       op=ALU.mult), reads=["ps5", "rzA"], writes=[f"zo{j}"])
                tr.op("act", lambda e: e.activation(out=rz[0:64, :], in_=PS[6][0:64, :], func=AF.Ln),
                      reads=["ps6"], writes=["rzB"])
                tr.op("act", lambda e: e.activation(out=rz[0:64, :], in_=rz[0:64, :], func=AF.Exp, scale=-1.0),
                      reads=["rzB"], writes=["rzB"])
                tr.op("dve", lambda e: e.tensor_tensor(out=zo[64:128, j, :], in0=PS[6][64:128, :], in1=rz[0:64, :],
                                                       op=ALU.mult), reads=["ps6", "rzB"], writes=[f"zo{j}"])
                tr.op("act", lambda e: e.activation(out=sq[:, j, :], in_=zo[:, j, :], func=AF.Square),
                      reads=[f"zo{j}"], writes=[f"sq{j}"])
            if g + 1 < NG:
                stage2(g + 1, 3)
            for j in range(4):
                tr.op("pe", lambda e, j=j: e.matmul(PS[0][:, :], lhsT=k.ones[:, :], rhs=sq[:, j, :],
                                                    start=(j == 0), stop=(j == 3)),
                      reads=["ones", f"sq{j}"], writes=["ps0"])
            rstd_tile(tr, rstd, PS[0], "ps0", 512)
            for j in range(4):
                tr.op("dve", lambda e, j=j: e.scalar_tensor_tensor(
                    out=yT[:, 4 + j, :], in0=zo[:, j, :], scalar=again[:, j:j + 1], in1=rstd[:, :],
                    op0=ALU.mult, op1=ALU.mult), reads=[f"zo{j}", "again", "rstd"], writes=[f"yT{4 + j}"])
            ykeys = [f"yT{c}" for c in range(8)]
            fbuf = [(xr[0], "xr0"), (xr[1], "xr1"), (xt[0], "xt0"), (xt[1], "xt1")]
            for t in (2, 3):
                tt = 4 * g + t
                xs, xk = fbuf[t]
                tr.dma("sp", xs[:], X_in[tt * 128:(tt + 1) * 128, :], reads=[f"{X_in.name}.{tt}"], writes=[xk],
                       semkey=pf + xk)
            for t in range(4):
                tt = 4 * g + t
                xs, xk = fbuf[t]
                for half in range(2):
                    bank, bk = PS[(2 * t + half) % 4], f"ps{(2 * t + half) % 4}"
                    for c in range(8):
                        tr.op("pe", lambda e, c=c, t=t, half=half, bank=bank: e.matmul(
                            bank[:, :], lhsT=yT[:, c, t * 128:(t + 1) * 128], rhs=w_out[:, c, half * 512:(half + 1) * 512],
                            start=(c == 0), stop=(c == 7)), reads=["w_out"] + ykeys, writes=[bk])
                    tr.op("dve", lambda e, xs=xs, half=half, bank=bank: e.tensor_tensor(
                        out=xs[:, half * 512:(half + 1) * 512], in0=bank[:, :], in1=xs[:, half * 512:(half + 1) * 512],
                        op=ALU.add), reads=[bk, xk], writes=[xk])
                tr.dma("sp", X_out[tt * 128:(tt + 1) * 128, :], xs[:], reads=[xk], writes=[f"{X_out.name}.{tt}"],
                       semkey=pf + "st" + xk)


def host_consts():
    c = {}
    c["maskT"] = mult_mask_table()
    c["ident"] = np.eye(128, dtype=np.float32).astype(ml_dtypes.bfloat16)
    c["identf"] = np.eye(128, dtype=np.float32)
    return c


def rc16_table(seq_start_own):
    t = np.arange(16)
    rows = []
    for which in range(2):
        vals = []
        for gi in range(4):
            w = 2 << gi
            if which == 0 or seq_start_own:
                cnt = np.minimum(t + 1, w)
            else:
                cnt = np.full(16, w)
            vals.append((1.0 / cnt).astype(np.float32))
        rows.append(np.concatenate(vals))
    return np.stack(rows).astype(np.float32)


def make_in_maps(inputs):
    x = np.ascontiguousarray(inputs["x"], dtype=np.float32)
    B = x.shape[0]
    consts = host_consts()
    shared = dict(consts)
    for name in ("norm_mix", "w_in", "w_out", "norm_ffn", "ffn_wg", "ffn_wu", "ffn_wd", "w_router",
                 "moe_wg", "moe_wu", "moe_wd"):
        shared[name] = np.ascontiguousarray(inputs[name], dtype=np.float32)
    shared["final_norm"] = np.ascontiguousarray(inputs["final_norm"], dtype=np.float32).reshape(1, D)
    shared["w_pool"] = np.ascontiguousarray(np.transpose(inputs["w_pool"], (0, 2, 1, 3)), dtype=np.float32)
    shared["pool_scale"] = np.ascontiguousarray(
        np.transpose(np.asarray(inputs["pool_scale"]).reshape(2, 4, 128), (0, 2, 1)), dtype=np.float32)
    shared["attn_gain"] = np.ascontiguousarray(
        np.transpose(np.asarray(inputs["attn_gain"]).reshape(2, 4, 128), (0, 2, 1)), dtype=np.float32)
    in_maps = []
    for c in range(8):
        b, half = c // 2, c % 2
        m = dict(shared)
        if half == 0:
            xl = np.concatenate([np.zeros((OWN0, D), np.float32), x[b, :OWN0]], axis=0)
        else:
            xl = x[b]
        m["x_loc"] = np.ascontiguousarray(xl)
        m["rc16"] = rc16_table(half == 0)
        m["cflag"] = np.full((128, 1), float(half), np.float32)
        in_maps.append(m)
    return in_maps


_NC_CACHE = {}


def kernel(**inputs):
    if "full" not in _NC_CACHE:
        _NC_CACHE["full"] = build(FULL_PHASES)
    nc = _NC_CACHE["full"]
    in_maps = make_in_maps(inputs)
    res = run_bass_kernel_spmd(nc, in_maps, core_ids=list(range(8)))
    out = np.empty((4, SEQ, D), np.float32)
    for c in range(8):
        b, half = c // 2, c % 2
        out[b, half * OWN0:(half + 1) * OWN0] = res.results[c]["out"]
    return out


def ffn_phase(k, l, X_in, X_out, lo, hi):
    from contextlib import ExitStack
    nc, tr = k.nc, k.tr
    pf = f"f{l}_"
    moe = (l == 1)
    final = (l == 1)
    SGT = 16
    with ExitStack() as es:
        sbt = lambda name, shape, dt: es.enter_context(nc.sbuf_tensor(pf + name, list(shape), dt))
        acc = sbt("acc", [128, SGT, D], F32)
        h2T = sbt("h2T", [128, 8, SGT * 128], BF16)
        gffn = sbt("gffn", [128, D], F32)
        wg_sb = [sbt(f"wg{i}", [128, 8, 512], BF16) for i in range(2)]
        wu_sb = [sbt(f"wu{i}", [128, 8, 512], BF16) for i in range(2)]
        wd_sb = [sbt(f"wd{i}", [128, 4, D], BF16) for i in range(2)]
        actT = sbt("actT", [128, 4, SGT * 128], BF16)
        sgt = [sbt(f"sgt{i}", [128, 512], F32) for i in range(2)]
        hb2 = [sbt(f"hb{i}", [128, D], BF16) for i in range(2)]
        junk = sbt("junk", [128, D], BF16)
        st2 = sbt("st2", [128, 2, 8], F32)
        st = st2[:, 0, :]
        PS, psT = k.ps, k.psT
        tr.dma("sp", gffn[:], k.norm_ffn[l:l + 1, :].partition_broadcast(128), writes=["gffn"], semkey="fc")
        if moe:
            gate = sbt("gate", [128, SGT, NE], F32)
            hf2 = [sbt(f"hf{i}", [128, D], F32) for i in range(2)]
            hT32 = sbt("hT32", [128, 8, 128], F32)
            wr = sbt("wr", [128, 8, NE], F32)
            gt = sbt("gt", [128, 8, NE], F32)
            gfin = sbt("gfin", [128, D], F32)
            tr.dma("sp", wr[:], k.w_router[0].rearrange("(c p) e -> p c e", p=128), writes=["wr"], semkey="fc")
            tr.dma("sp", gfin[:], k.final_norm[0:1, :].partition_broadcast(128), writes=["gfin"], semkey="fc")
        blocks = []
        if moe:
            for e_ in range(NE):
                for f0 in range(0, D_FFE, 512):
                    blocks.append((k.moe_wg[0, e_], k.moe_wu[0, e_], k.moe_wd[0, e_], f0, 4, e_))
        else:
            for f0 in range(0, D_FF, 512):
                blocks.append((k.ffn_wg[0], k.ffn_wu[0], k.ffn_wd[0], f0, min(4, (D_FF - f0) // 128), None))
        nb = len(blocks)
        bctr = [0]

        def load_block(bi, par):
            wg_ap, wu_ap, wd_ap, f0, nf, _ = blocks[bi]
            fw = nf * 128
            for (dst, src, key) in ((wg_sb[par], wg_ap, f"wg{par}"), (wu_sb[par], wu_ap, f"wu{par}")):
                for c0 in (0, 4):
                    tr.dma("pool", dst[:, c0:c0 + 4, 0:fw],
                           src[c0 * 128:(c0 + 4) * 128, f0:f0 + fw].rearrange("(c p) f -> p c f", p=128),
                           writes=[key], semkey=pf + key)
            tr.dma("pool", wd_sb[par][:, 0:nf, :], wd_ap[f0:f0 + fw, :].rearrange("(c p) n -> p c n", p=128),
                   writes=[f"wd{par}"], semkey=pf + f"wd{par}")

        for sg0 in range(lo // 128, hi // 128, SGT):
            load_block(0, bctr[0] % 2)
            def stage1(t):
                tt = sg0 + t
                ak = f"acc{t}"
                par = t % 2
                kp = f"p{par}"
                stp = st2[:, par, :]
                tr.dma("sp", acc[:, t, :], X_in[tt * 128:(tt + 1) * 128, :], reads=[f"{X_in.name}.{tt}"], writes=[ak],
                       semkey=pf + f"acc{t % 4}")
                tr.op("dve", lambda e: e.memset(stp[:, 0:1], 0.0), writes=[kp + "st0"])
                tr.op("act", lambda e: e.activation(out=junk[:], in_=acc[:, t, :], func=AF.Square,
                                                    accum_out=stp[:, 0:1]), reads=[ak], writes=["junk", kp + "st0"])
                rms_rstd(tr, stp, D, kp)
                tr.op("dve", lambda e: e.scalar_tensor_tensor(
                    out=hb2[par][:], in0=acc[:, t, :], scalar=stp[:, 2:3], in1=gffn[:], op0=ALU.mult, op1=ALU.mult),
                    reads=[ak, kp + "st2", "gffn"], writes=[f"hb{par}"])
                if moe:
                    tr.op("dve", lambda e: e.scalar_tensor_tensor(
                        out=hf2[par][:], in0=acc[:, t, :], scalar=stp[:, 2:3], in1=gffn[:], op0=ALU.mult, op1=ALU.mult),
                        reads=[ak, kp + "st2", "gffn"], writes=[f"hf{par}"])

            def stage2(t):
                par = t % 2
                kp = f"p{par}"
                stp = st2[:, par, :]
                hbp, hbk = hb2[par], f"hb{par}"
                for c in range(8):
                    tr.op("pe", lambda e, c=c: e.transpose(out=psT[:, c, :], in_=hbp[:, c * 128:(c + 1) * 128],
                                                           identity=k.idb[:]), reads=[hbk, "idb"], writes=["psT"])
                tr.op("act", lambda e: e.copy(out=h2T[:, :, t * 128:(t + 1) * 128], in_=psT[:, :, :]),
                      reads=["psT"], writes=[f"h2T{t // 4}"])
                if moe:
                    hfp, hfk = hf2[par], f"hf{par}"
                    for c in range(8):
                        bank, bk = PS[5 + c // 4], f"ps{5 + c // 4}"
                        tr.op("pe", lambda e, c=c, bank=bank: e.transpose(
                            out=bank[:, (c % 4) * 128:(c % 4 + 1) * 128], in_=hfp[:, c * 128:(c + 1) * 128],
                            identity=k.idf[:]), reads=[hfk, "idf"], writes=[bk])
                    tr.op("act", lambda e: e.copy(out=hT32[:, 0:4, :], in_=PS[5][:, :].rearrange("p (c t) -> p c t", t=128)),
                          reads=["ps5"], writes=["hT32"])
                    tr.op("dve", lambda e: e.tensor_copy(out=hT32[:, 4:8, :], in_=PS[6][:, :].rearrange("p (c t) -> p c t", t=128)),
                          reads=["ps6"], writes=["hT32"])
                    for c in range(8):
                        tr.op("pe", lambda e, c=c: e.matmul(PS[4][:, 0:NE], lhsT=hT32[:, c, :], rhs=wr[:, c, :],
                                                            start=(c == 0), stop=(c == 7)),
                              reads=["hT32", "wr"], writes=["ps4"])
                    lg, eq1, lg2, sel, ex = gt[:, 0, :], gt[:, 1, :], gt[:, 2, :], gt[:, 3, :], gt[:, 4, :]
                    S = lambda i: stp[:, i:i + 1]
                    tr.op("dve", lambda e: e.tensor_copy(out=lg, in_=PS[4][:, 0:NE]), reads=["ps4"], writes=["lg"])
                    tr.op("dve", lambda e: e.reduce_max(out=S(3), in_=lg, axis=AX.X), reads=["lg"], writes=[kp + "st3"])
                    tr.op("dve", lambda e: e.tensor_scalar(out=eq1, in0=lg, scalar1=S(3), scalar2=None,
                                                            op0=ALU.is_equal), reads=["lg", kp + "st3"], writes=["eq1"])
                    tr.op("dve", lambda e: e.scalar_tensor_tensor(out=lg2, in0=eq1, scalar=-1e30, in1=lg,
                                                                   op0=ALU.mult, op1=ALU.add),
                          reads=["eq1", "lg"], writes=["lg2"])
                    tr.op("dve", lambda e: e.reduce_max(out=S(4), in_=lg2, axis=AX.X), reads=["lg2"], writes=[kp + "st4"])
                    tr.op("dve", lambda e: e.tensor_scalar(out=sel, in0=lg, scalar1=S(4), scalar2=None,
                                                            op0=ALU.is_ge), reads=["lg", kp + "st4"], writes=["sel"])
                    tr.op("dve", lambda e: e.tensor_scalar(out=S(5), in0=S(3), scalar1=-1.0, scalar2=None,
                                                            op0=ALU.mult), reads=[kp + "st3"], writes=[kp + "st5"])
                    tr.op("act", lambda e: e.activation(out=ex, in_=lg, func=AF.Exp, bias=S(5), scale=1.0),
                          reads=["lg", kp + "st5"], writes=["ex"])
                    tr.op("dve", lambda e: e.tensor_tensor(out=ex, in0=ex, in1=sel, op=ALU.mult),
                          reads=["ex", "sel"], writes=["ex"])
                    tr.op("dve", lambda e: e.reduce_sum(out=S(6), in_=ex, axis=AX.X), reads=["ex"], writes=[kp + "st6"])
                    tr.op("dve", lambda e: e.reciprocal(out=S(7), in_=S(6)), reads=[kp + "st6"], writes=[kp + "st7"])
                    tr.op("dve", lambda e: e.tensor_scalar(out=gate[:, t, :], in0=ex, scalar1=S(7), scalar2=None,
                                                            op0=ALU.mult), reads=["ex", kp + "st7"], writes=[f"gate{t}"])

            for t in range(SGT):
                stage1(t)
                if t >= 1:
                    stage2(t - 1)
            stage2(SGT - 1)
            hkeys = [f"h2T{i}" for i in range(4)]
            it = 0
            for bi in range(nb):
                par = bctr[0] % 2
                if bi + 1 < nb:
                    load_block(bi + 1, (bctr[0] + 1) % 2)
                _, _, _, f0, nf, ex_i = blocks[bi]
                for fc in range(nf):
                    for gq in range(4):
                        Gb, Ub = PS[2 * (it % 2)], PS[2 * (it % 2) + 1]
                        gk, uk = f"ps{2 * (it % 2)}", f"ps{2 * (it % 2) + 1}"
                        for (bank, bkey, wsb, wkey) in ((Gb, gk, wg_sb[par], f"wg{par}"), (Ub, uk, wu_sb[par], f"wu{par}")):
                            for c in range(8):
                                tr.op("pe", lambda e, c=c, bank=bank, wsb=wsb, fc=fc, gq=gq: e.matmul(
                                    bank[:, :], lhsT=wsb[:, c, fc * 128:(fc + 1) * 128], rhs=h2T[:, c, gq * 512:(gq + 1) * 512],
                                    start=(c == 0), stop=(c == 7)), reads=[wkey, f"h2T{gq}"], writes=[bkey])
                        sg_, sk = sgt[it % 2], f"sgt{it % 2}"
                        tr.op("act", lambda e, Gb=Gb, sg_=sg_: e.activation(out=sg_[:, :], in_=Gb[:, :], func=AF.Silu),
                              reads=[gk], writes=[sk])
                        tr.op("dve", lambda e, Ub=Ub, sg_=sg_, fc=fc, gq=gq: e.tensor_tensor(
                            out=actT[:, fc, gq * 512:(gq + 1) * 512], in0=Ub[:, :], in1=sg_[:, :], op=ALU.mult),
                            reads=[uk, sk], writes=[f"actT{gq}"])
                        it += 1
                for t in range(SGT):
                    for half in range(2):
                        bank, bk = PS[4 + (t * 2 + half) % NB2], f"ps{4 + (t * 2 + half) % NB2}"
                        for fc in range(nf):
                            tr.op("pe", lambda e, fc=fc, t=t, half=half, bank=bank: e.matmul(
                                bank[:, :], lhsT=actT[:, fc, t * 128:(t + 1) * 128],
                                rhs=wd_sb[par][:, fc, half * 512:(half + 1) * 512], start=(fc == 0), stop=(fc == nf - 1)),
                                reads=[f"actT{t // 4}", f"wd{par}"], writes=[bk])
                        asl = acc[:, t, half * 512:(half + 1) * 512]
                        if moe:
                            tr.op("dve", lambda e, bank=bank, asl=asl, t=t, ex_i=ex_i: e.scalar_tensor_tensor(
                                out=asl, in0=bank[:, :], scalar=gate[:, t, ex_i:ex_i + 1], in1=asl,
                                op0=ALU.mult, op1=ALU.add), reads=[bk, f"acc{t}", f"gate{t}"], writes=[f"acc{t}"])
                        else:
                            tr.op("dve", lambda e, bank=bank, asl=asl: e.tensor_tensor(out=asl, in0=bank[:, :], in1=asl,
                                                                                      op=ALU.add),
                                  reads=[bk, f"acc{t}"], writes=[f"acc{t}"])
                bctr[0] += 1
            for t in range(SGT):
                tt = sg0 + t
                ak = f"acc{t}"
                if final:
                    tr.op("dve", lambda e: e.memset(st[:, 0:1], 0.0), writes=["p0st0"])
                    tr.op("act", lambda e, t=t: e.activation(out=junk[:], in_=acc[:, t, :], func=AF.Square,
                                                             accum_out=st[:, 0:1]), reads=[ak], writes=["junk", "p0st0"])
                    rms_rstd(tr, st, D, "p0")
                    tr.op("dve", lambda e, t=t: e.scalar_tensor_tensor(
                        out=acc[:, t, :], in0=acc[:, t, :], scalar=st[:, 2:3], in1=gfin[:], op0=ALU.mult, op1=ALU.mult),
                        reads=[ak, "p0st2", "gfin"], writes=[ak])
                    ro = tt - lo // 128
                else:
                    ro = tt
                tr.dma("sp", X_out[ro * 128:(ro + 1) * 128, :], acc[:, t, :], reads=[ak], writes=[f"{X_out.name}.{ro}"],
                       semkey=pf + f"st{t % 4}")
```
